# Optimizing a Trainium2 kernel written in Bass

```python
import math
import jax, jax.numpy as jnp
from jax import lax
import numpy as np


D_MODEL = 1024
BATCH = 4
SEQ = 8192
DEPTH = 4

MIX_WIDTH = D_MODEL // 2
N_BRANCH = 4
EPS = 1e-6

RWKV_HEAD_DIM = 64
RWKV_HEADS = MIX_WIDTH // RWKV_HEAD_DIM
RWKV_DECAY_LORA = 64
RWKV_A_LORA = 64
RWKV_G_LORA = 128
RWKV_DECAY_SCALE = 0.6065306597
RWKV_GN_EPS = 64e-5
GLA_HEADS = 4
GLA_DK = MIX_WIDTH // 2
GLA_DV = MIX_WIDTH
GLA_GATE_LORA = 16
GLA_TAU = 16.0
GLA_CHUNK = 16
MLSTM_HEADS = 4
MLSTM_DQK = MIX_WIDTH // 2
MLSTM_DV = MIX_WIDTH
MLSTM_CONV = 4
MLSTM_CHUNK = 64
SSD_HEADS = 8
SSD_HEAD_DIM = MIX_WIDTH // SSD_HEADS
SSD_GROUPS = 2
SSD_HPG = SSD_HEADS // SSD_GROUPS
SSD_STATE = 128
SSD_CONV = 4
SSD_CHUNK = 128
SSD_CONV_DIM = MIX_WIDTH + 2 * SSD_GROUPS * SSD_STATE
D_FF = 2816
FFN_CONV = 3

RWKV_IN = 3 * MIX_WIDTH + RWKV_DECAY_LORA + RWKV_A_LORA + RWKV_G_LORA
GLA_IN = 2 * GLA_DK + 2 * GLA_DV + GLA_GATE_LORA
MLSTM_IN = 2 * MLSTM_DQK + 2 * MLSTM_DV + 2 * MLSTM_HEADS
SSD_IN = MIX_WIDTH + SSD_CONV_DIM + SSD_HEADS
GATE_IN = N_BRANCH * D_MODEL
MIXER_IN_SIZES = (RWKV_IN, GLA_IN, MLSTM_IN, SSD_IN, GATE_IN)
D_IN = RWKV_IN + GLA_IN + MLSTM_IN + SSD_IN + GATE_IN

kernel_name = 'hybrid_parallel_mixer_trunk'


def _split(a, sizes):
    idx = np.cumsum(np.array(sizes))[:-1].tolist()
    return jnp.split(a, idx, axis=-1)


def rms_norm(x, g):
    xf = x.astype(jnp.float32)
    return xf * lax.rsqrt(jnp.mean(xf * xf, axis=-1, keepdims=True) + EPS) * g


def head_rms_norm(y, g):
    y = y.astype(jnp.float32)
    return y * lax.rsqrt(jnp.mean(y * y, axis=-1, keepdims=True) + EPS) * g


def head_group_norm(y, g, b, eps):
    y = y.astype(jnp.float32)
    yc = y - jnp.mean(y, axis=-1, keepdims=True)
    return yc * lax.rsqrt(jnp.mean(yc * yc, axis=-1, keepdims=True) + eps) * g + b


def causal_dwconv(x, w, b):
    k, c = w.shape
    y = lax.conv_general_dilated(x, w[:, None, :].astype(x.dtype), window_strides=(1,),
                                 padding=((k - 1, 0),), dimension_numbers=('NWC', 'WIO', 'NWC'),
                                 feature_group_count=c)
    return y + b


def token_shift(f, mu):
    prev = jnp.pad(f, ((0, 0), (1, 0), (0, 0)))[:, :-1]
    return f + mu * (prev - f)


def to_chunks(a, size):
    bsz, t = a.shape[:2]
    return jnp.moveaxis(a.reshape((bsz, t // size, size) + a.shape[2:]), 1, 0)


def from_chunks(a):
    nc, bsz, size = a.shape[:3]
    return jnp.moveaxis(a, 0, 1).reshape((bsz, nc * size) + a.shape[3:])


def causal_mask(size):
    return jnp.tril(jnp.ones((size, size), dtype=bool))


def rwkv7_mix(fa, mu, w0, w_up, a0, a_up, g_up, k_k, k_a, r_k, gn_g, gn_b):
    bsz, t, _ = fa.shape
    fa = token_shift(fa.astype(jnp.float32), mu)
    r, k, v, w_lo, a_lo, g_lo = _split(fa, (MIX_WIDTH, MIX_WIDTH, MIX_WIDTH, RWKV_DECAY_LORA, RWKV_A_LORA, RWKV_G_LORA))
    heads = lambda z: z.reshape(bsz, t, RWKV_HEADS, RWKV_HEAD_DIM)
    log_decay = -RWKV_DECAY_SCALE * jax.nn.sigmoid(w0 + jnp.tanh(w_lo) @ w_up)
    a = jax.nn.sigmoid(a0 + a_lo @ a_up)
    g = jax.nn.sigmoid(g_lo) @ g_up
    kk = heads(k * k_k)
    kk = kk / jnp.maximum(jnp.sqrt(jnp.sum(kk * kk, axis=-1, keepdims=True)), 1e-12)
    k = k * (1.0 + (a - 1.0) * k_a)
    r, k, v, a, decay = heads(r), heads(k), heads(v), heads(a), jnp.exp(heads(log_decay))

    def step(s, inp):
        r_t, w_t, k_t, v_t, kk_t, a_t = inp
        sa = jnp.einsum('bhvk,bhk->bhv', s, -kk_t)
        s = (s * w_t[:, :, None, :] + sa[..., None] * (kk_t * a_t)[:, :, None, :]
             + v_t[..., None] * k_t[:, :, None, :])
        return s, jnp.einsum('bhvk,bhk->bhv', s, r_t)

    time_major = lambda z: jnp.moveaxis(z, 1, 0)
    s0 = jnp.zeros((bsz, RWKV_HEADS, RWKV_HEAD_DIM, RWKV_HEAD_DIM), jnp.float32)
    _, y = lax.scan(step, s0, tuple(time_major(z) for z in (r, decay, k, v, kk, a)))
    y = jnp.moveaxis(y, 0, 1)
    y = head_group_norm(y, gn_g.reshape(RWKV_HEADS, RWKV_HEAD_DIM), gn_b.reshape(RWKV_HEADS, RWKV_HEAD_DIM), RWKV_GN_EPS)
    y = y + jnp.sum(r * k * r_k, axis=-1, keepdims=True) * v
    return y.reshape(bsz, t, MIX_WIDTH) * g


def gla_mix(fb, f_up, f_bias, norm_g):
    bsz, t, _ = fb.shape
    dk, dv = GLA_DK // GLA_HEADS, GLA_DV // GLA_HEADS
    q, k, v, f_lo, og = _split(fb.astype(jnp.float32), (GLA_DK, GLA_DK, GLA_DV, GLA_GATE_LORA, GLA_DV))
    hk = lambda z: z.reshape(bsz, t, GLA_HEADS, dk)
    log_alpha = hk(jax.nn.log_sigmoid(f_lo @ f_up + f_bias) / GLA_TAU)
    q = hk(q) * dk ** -0.5
    k = hk(k)
    v = v.reshape(bsz, t, GLA_HEADS, dv)
    mask = causal_mask(GLA_CHUNK)

    def step(s, inp):
        q_c, k_c, v_c, la_c = inp
        b = jnp.cumsum(la_c, axis=1)
        b_last = b[:, -1]
        qd = q_c * jnp.exp(b)
        kd = k_c * jnp.exp(-b)
        att = jnp.where(mask, jnp.einsum('blhd,bshd->bhls', qd, kd), 0.0)
        o = jnp.einsum('bhls,bshe->blhe', att, v_c) + jnp.einsum('blhd,bhde->blhe', qd, s)
        s = jnp.exp(b_last)[..., None] * s + jnp.einsum('bshd,bshe->bhde', k_c * jnp.exp(b_last[:, None] - b), v_c)
        return s, o

    s0 = jnp.zeros((bsz, GLA_HEADS, dk, dv), jnp.float32)
    _, o = lax.scan(step, s0, tuple(to_chunks(z, GLA_CHUNK) for z in (q, k, v, log_alpha)))
    o = head_rms_norm(from_chunks(o), norm_g)
    return o.reshape(bsz, t, GLA_DV) * jax.nn.silu(og)


def mlstm_mix(fc, conv_w, conv_b, i_bias, f_bias, norm_g):
    bsz, t, _ = fc.shape
    dqk, dv = MLSTM_DQK // MLSTM_HEADS, MLSTM_DV // MLSTM_HEADS
    qk, v, i_pre, f_pre, og = _split(fc.astype(jnp.float32), (2 * MLSTM_DQK, MLSTM_DV, MLSTM_HEADS, MLSTM_HEADS, MLSTM_DV))
    q, k = _split(jax.nn.silu(causal_dwconv(qk, conv_w, conv_b)), (MLSTM_DQK, MLSTM_DQK))
    q = q.reshape(bsz, t, MLSTM_HEADS, dqk)
    k = k.reshape(bsz, t, MLSTM_HEADS, dqk) * dqk ** -0.5
    v = v.reshape(bsz, t, MLSTM_HEADS, dv)
    log_i = i_pre + i_bias
    log_f = jax.nn.log_sigmoid(f_pre + f_bias)
    mask = causal_mask(MLSTM_CHUNK)

    def step(carry, inp):
        c, n, m = carry
        q_c, k_c, v_c, li_c, lf_c = inp
        b = jnp.swapaxes(jnp.cumsum(lf_c, axis=1), 1, 2)
        li_c = jnp.swapaxes(li_c, 1, 2)
        log_d = jnp.where(mask, b[..., :, None] - b[..., None, :] + li_c[..., None, :], -jnp.inf)
        m_t = jnp.maximum(b + m[..., None], jnp.max(log_d, axis=-1))
        d = jnp.exp(log_d - m_t[..., None])
        inter = jnp.exp(b + m[..., None] - m_t)
        sc = jnp.einsum('blhd,bshd->bhls', q_c, k_c) * d
        num = (jnp.einsum('bhls,bshe->blhe', sc, v_c)
               + jnp.einsum('blhd,bhde->blhe', q_c, c) * jnp.swapaxes(inter, 1, 2)[..., None])
        den = jnp.sum(sc, axis=-1) + inter * jnp.einsum('blhd,bhd->bhl', q_c, n)
        h = num / jnp.swapaxes(jnp.maximum(jnp.abs(den), jnp.exp(-m_t)), 1, 2)[..., None]
        w_last, f_last = d[..., -1, :], inter[..., -1]
        c = f_last[..., None, None] * c + jnp.einsum('bhs,bshd,bshe->bhde', w_last, k_c, v_c)
        n = f_last[..., None] * n + jnp.einsum('bhs,bshd->bhd', w_last, k_c)
        return (c, n, m_t[..., -1]), h

    carry0 = (jnp.zeros((bsz, MLSTM_HEADS, dqk, dv), jnp.float32),
              jnp.zeros((bsz, MLSTM_HEADS, dqk), jnp.float32),
              jnp.zeros((bsz, MLSTM_HEADS), jnp.float32))
    _, h = lax.scan(step, carry0, tuple(to_chunks(z, MLSTM_CHUNK) for z in (q, k, v, log_i, log_f)))
    h = head_rms_norm(from_chunks(h), norm_g.reshape(MLSTM_HEADS, dv))
    return h.reshape(bsz, t, MLSTM_DV) * jax.nn.sigmoid(og)


def ssd_mix(fd, conv_w, conv_b, dt_bias, a_log, d_skip, norm_g):
    bsz, t, _ = fd.shape
    z, xbc, dt = _split(fd.astype(jnp.float32), (MIX_WIDTH, SSD_CONV_DIM, SSD_HEADS))
    x, bm, cm = _split(jax.nn.silu(causal_dwconv(xbc, conv_w, conv_b)),
                       (MIX_WIDTH, SSD_GROUPS * SSD_STATE, SSD_GROUPS * SSD_STATE))
    x = x.reshape(bsz, t, SSD_GROUPS, SSD_HPG, SSD_HEAD_DIM)
    bm = bm.reshape(bsz, t, SSD_GROUPS, SSD_STATE)
    cm = cm.reshape(bsz, t, SSD_GROUPS, SSD_STATE)
    dt = jax.nn.softplus(dt + dt_bias).reshape(bsz, t, SSD_GROUPS, SSD_HPG)
    a = -jnp.exp(a_log).reshape(SSD_GROUPS, SSD_HPG)
    mask = causal_mask(SSD_CHUNK)

    def step(s, inp):
        x_c, dt_c, b_c, c_c = inp
        acum = jnp.cumsum(dt_c * a, axis=1)
        ah = jnp.moveaxis(acum, 1, -1)
        seg = jnp.exp(jnp.where(mask, ah[..., :, None] - ah[..., None, :], -jnp.inf))
        xdt = x_c * dt_c[..., None]
        cb = jnp.einsum('blgn,bsgn->bgls', c_c, b_c)
        y = (jnp.einsum('bgls,bgrls,bsgrp->blgrp', cb, seg, xdt)
             + jnp.einsum('blgn,bgrpn,blgr->blgrp', c_c, s, jnp.exp(acum)))
        last = acum[:, -1]
        s = (jnp.exp(last)[..., None, None] * s
             + jnp.einsum('bsgn,bsgr,bsgrp->bgrpn', b_c, jnp.exp(last[:, None] - acum), xdt))
        return s, y

    s0 = jnp.zeros((bsz, SSD_GROUPS, SSD_HPG, SSD_HEAD_DIM, SSD_STATE), jnp.float32)
    _, y = lax.scan(step, s0, tuple(to_chunks(zz, SSD_CHUNK) for zz in (x, dt, bm, cm)))
    y = from_chunks(y) + x * d_skip.reshape(SSD_GROUPS, SSD_HPG)[..., None]
    return rms_norm(y.reshape(bsz, t, MIX_WIDTH) * jax.nn.silu(z), norm_g)


def setup_inputs(seed: int = 0) -> dict:
    key = jax.random.key(seed)
    ks = iter(jax.random.split(key, 48))

    def nrm(shape, scale):
        return scale * jax.random.normal(next(ks), shape, jnp.float32)

    def gain(shape):
        return 1.0 + nrm(shape, 0.02)

    L = DEPTH
    dt0 = jnp.exp(jax.random.uniform(next(ks), (L, SSD_HEADS), jnp.float32, math.log(1e-3), math.log(1e-1)))
    return {
        'x': nrm((BATCH, SEQ, D_MODEL), 1.0),
        'mix_norm_g': gain((L, D_MODEL)),
        'w_in': nrm((L, D_MODEL, D_IN), D_MODEL ** -0.5),
        'rwkv_mu': jax.random.uniform(next(ks), (L, RWKV_IN), jnp.float32),
        'rwkv_w0': nrm((L, MIX_WIDTH), 1.0),
        'rwkv_w_up': nrm((L, RWKV_DECAY_LORA, MIX_WIDTH), RWKV_DECAY_LORA ** -0.5),
        'rwkv_a0': nrm((L, MIX_WIDTH), 0.5),
        'rwkv_a_up': nrm((L, RWKV_A_LORA, MIX_WIDTH), 0.5 * RWKV_A_LORA ** -0.5),
        'rwkv_g_up': nrm((L, RWKV_G_LORA, MIX_WIDTH), RWKV_G_LORA ** -0.5),
        'rwkv_k_k': 0.85 + nrm((L, MIX_WIDTH), 0.02),
        'rwkv_k_a': 1.0 + nrm((L, MIX_WIDTH), 0.02),
        'rwkv_r_k': nrm((L, RWKV_HEADS, RWKV_HEAD_DIM), 0.1),
        'rwkv_gn_g': gain((L, MIX_WIDTH)),
        'rwkv_gn_b': nrm((L, MIX_WIDTH), 0.02),
        'gla_f_up': nrm((L, GLA_GATE_LORA, GLA_DK), GLA_GATE_LORA ** -0.5),
        'gla_f_bias': nrm((L, GLA_DK), 0.5),
        'gla_norm_g': gain((L, GLA_DV // GLA_HEADS)),
        'mlstm_conv_w': nrm((L, MLSTM_CONV, 2 * MLSTM_DQK), MLSTM_CONV ** -0.5),
        'mlstm_conv_b': nrm((L, 2 * MLSTM_DQK), 0.02),
        'mlstm_i_bias': nrm((L, MLSTM_HEADS), 0.1),
        'mlstm_f_bias': jnp.linspace(3.0, 6.0, MLSTM_HEADS) + nrm((L, MLSTM_HEADS), 0.1),
        'mlstm_norm_g': gain((L, MLSTM_DV)),
        'ssd_conv_w': nrm((L, SSD_CONV, SSD_CONV_DIM), SSD_CONV ** -0.5),
        'ssd_conv_b': nrm((L, SSD_CONV_DIM), 0.02),
        'ssd_dt_bias': dt0 + jnp.log(-jnp.expm1(-dt0)),
        'ssd_a_log': jnp.log(jax.random.uniform(next(ks), (L, SSD_HEADS), jnp.float32, 1.0, 16.0)),
        'ssd_d': gain((L, SSD_HEADS)),
        'ssd_norm_g': gain((L, MIX_WIDTH)),
        'branch_proj': nrm((L, N_BRANCH, MIX_WIDTH, D_MODEL), MIX_WIDTH ** -0.5),
        'w_out': nrm((L, D_MODEL, D_MODEL), D_MODEL ** -0.5),
        'ffn_norm_g': gain((L, D_MODEL)),
        'ffn_up': nrm((L, D_MODEL, 2 * D_FF), D_MODEL ** -0.5),
        'ffn_conv_w': nrm((L, FFN_CONV, 2 * D_FF), FFN_CONV ** -0.5),
        'ffn_conv_b': nrm((L, 2 * D_FF), 0.02),
        'ffn_down': nrm((L, D_FF, D_MODEL), D_FF ** -0.5),
        'final_norm_g': gain((D_MODEL,)),
    }


def reference(x, mix_norm_g, w_in, rwkv_mu, rwkv_w0, rwkv_w_up, rwkv_a0, rwkv_a_up, rwkv_g_up,
              rwkv_k_k, rwkv_k_a, rwkv_r_k, rwkv_gn_g, rwkv_gn_b, gla_f_up, gla_f_bias, gla_norm_g,
              mlstm_conv_w, mlstm_conv_b, mlstm_i_bias, mlstm_f_bias, mlstm_norm_g,
              ssd_conv_w, ssd_conv_b, ssd_dt_bias, ssd_a_log, ssd_d, ssd_norm_g,
              branch_proj, w_out, ffn_norm_g, ffn_up, ffn_conv_w, ffn_conv_b, ffn_down, final_norm_g):
    bsz, t, _ = x.shape
    h = x
    for l in range(DEPTH):
        u = rms_norm(h, mix_norm_g[l])
        fa, fb, fc, fd, fg = _split(u @ w_in[l], MIXER_IN_SIZES)
        branches = (
            rwkv7_mix(fa, rwkv_mu[l], rwkv_w0[l], rwkv_w_up[l], rwkv_a0[l], rwkv_a_up[l], rwkv_g_up[l],
                      rwkv_k_k[l], rwkv_k_a[l], rwkv_r_k[l], rwkv_gn_g[l], rwkv_gn_b[l]),
            gla_mix(fb, gla_f_up[l], gla_f_bias[l], gla_norm_g[l]),
            mlstm_mix(fc, mlstm_conv_w[l], mlstm_conv_b[l], mlstm_i_bias[l], mlstm_f_bias[l], mlstm_norm_g[l]),
            ssd_mix(fd, ssd_conv_w[l], ssd_conv_b[l], ssd_dt_bias[l], ssd_a_log[l], ssd_d[l], ssd_norm_g[l]),
        )
        gates = jax.nn.sigmoid(fg.astype(jnp.float32)).reshape(bsz, t, N_BRANCH, D_MODEL)
        merged = gates[:, :, 0] * (branches[0] @ branch_proj[l, 0])
        for i in range(1, N_BRANCH):
            merged = merged + gates[:, :, i] * (branches[i] @ branch_proj[l, i])
        h = h + merged @ w_out[l]
        u = rms_norm(h, ffn_norm_g[l])
        gate, val = _split(causal_dwconv(u @ ffn_up[l], ffn_conv_w[l], ffn_conv_b[l]), (D_FF, D_FF))
        h = h + (jax.nn.silu(gate) * val) @ ffn_down[l]
    return rms_norm(h, final_norm_g).astype(x.dtype)
```

```python
from concourse.bass_utils import run_bass_kernel_spmd


import contextlib
import numpy as np
import concourse.bass as bass
import concourse.mybir as mybir

F32 = mybir.dt.float32
BF16 = mybir.dt.bfloat16
AF = mybir.ActivationFunctionType
ALU = mybir.AluOpType
AX = mybir.AxisListType


class Tile:
    def __init__(self, t, name):
        self.t = t
        self.name = name
        self.lw = None
        self.rd = {}

    def __getitem__(self, idx):
        return self.t[idx]


class Rot:
    def __init__(self, tiles):
        self.tiles = tiles
        self.i = 0

    def next(self):
        t = self.tiles[self.i % len(self.tiles)]
        self.i += 1
        return t


class KB:
    ENGS = ("pe", "act", "dve", "pool", "sp")
    EPOCH = 8192
    EMAP = {"pe": "tensor", "act": "scalar", "dve": "vector", "pool": "gpsimd", "sp": "sync"}

    def __init__(self, nc, n_dma_slots=8):
        self.nc = nc
        self.es = contextlib.ExitStack()
        self.ops = {e: [] for e in self.ENGS}
        self.count = {e: 0 for e in self.ENGS}
        self.waited = {e: {} for e in self.ENGS}
        self.semobj = {}
        self.nslots = n_dma_slots
        self.dcount = {q: 0 for q in ("sp", "pool", "act")}
        self.tiles = {}
        self.ntiles = 0
        self.dma_toks = []

    def _reg(self, t, name):
        tl = Tile(t, name)
        self.tiles[name] = tl
        return tl

    def sb(self, shape, dtype, name=None):
        self.ntiles += 1
        name = name or f"t{self.ntiles}"
        return self._reg(self.es.enter_context(self.nc.sbuf_tensor(name, list(shape), dtype)), name)

    def ps(self, shape, dtype, name=None):
        self.ntiles += 1
        name = name or f"p{self.ntiles}"
        t = self._reg(self.es.enter_context(self.nc.psum_tensor(name, list(shape), dtype)), name)
        t.is_psum = True
        return t

    def dram(self, name, shape, dtype, kind):
        return self._reg(self.nc.dram_tensor(name, list(shape), dtype, kind=kind), name)

    def rot(self, n, shape, dtype, name, psum=False):
        mk = self.ps if psum else self.sb
        return Rot([mk(shape, dtype, f"{name}{i}") for i in range(n)])

    def tl(self, ap):
        return self.tiles[ap.name]

    def _deps(self, eng, reads, writes):
        need = {}

        def add(w, kind):
            if w is None:
                return
            key, val, weng = w
            if weng == eng and eng == "pe":
                return
            if need.get(key, 0) < val:
                need[key] = val

        for t in reads:
            add(t.lw, "raw")
        for t in writes:
            add(t.lw, "waw")
            for key, (val, weng) in t.rd.items():
                add((key, val, weng), "war")
        waits = []
        for key, val in need.items():
            if self.waited[eng].get(key, 0) < val:
                self.waited[eng][key] = val
                waits.append((key, val))
        return waits

    def _commit(self, tokn, reads, writes):
        key, val, eng = tokn
        for t in reads:
            if t.rd.get(key, (0, None))[0] < val:
                t.rd[key] = (val, eng)
        for t in writes:
            t.lw = tokn
            t.rd = {}

    def op(self, eng, fn, reads=(), writes=(), inc=True):
        reads = [self.tl(a) if not isinstance(a, Tile) else a for a in reads if a is not None and not isinstance(a, (int, float))]
        writes = [self.tl(a) if not isinstance(a, Tile) else a for a in writes]
        writes = writes + [t for t in reads if getattr(t, "is_psum", False) and t not in writes]
        waits = self._deps(eng, reads, writes)
        n = self.count[eng] + 1
        if inc:
            self.count[eng] = n
        key = ("e", eng, (n - 1) // self.EPOCH)
        val = (n - 1) % self.EPOCH + 1
        tokn = (key, val, eng)
        self._commit(tokn, reads, writes)
        self.ops[eng].append((waits, fn, key if inc else None, 1))

    def dma(self, q, out, in_, **kw):
        reads = [self.tl(in_)]
        writes = [self.tl(out)]
        i = self.dcount[q]
        self.dcount[q] = i + 1
        slot = i % self.nslots
        key = ("d", q, slot)
        prev = 16 * (i // self.nslots)
        waits = self._deps(q, reads, writes)
        if prev > 0 and self.waited[q].get(key, 0) < prev:
            self.waited[q][key] = prev
            waits.append((key, prev))
        tokn = (key, prev + 16, "dma_" + q)
        self._commit(tokn, reads, writes)

        def fn(e, out=out, in_=in_, kw=kw):
            return e.dma_start(out=out, in_=in_, **kw)

        self.ops[q].append((waits, fn, key, 16))
        self.dma_toks.append((q, tokn))
        return tokn

    def dump(self, name, ap):
        if not getattr(self, "debug", False):
            return
        if name in self.tiles:
            return
        d = self.dram(name, list(ap.shape), ap.dtype, "ExternalOutput")
        self.dma("sp", d[:], ap)

    def finish(self):
        for q in ("sp", "pool", "act"):
            waits = []
            last = {}
            for qq, (key, val, _) in self.dma_toks:
                if qq == q:
                    last[key] = max(last.get(key, 0), val)
            for key, val in last.items():
                if self.waited[q].get(key, 0) < val:
                    self.waited[q][key] = val
                    waits.append((key, val))
            if waits:
                self.ops[q].append((waits, None, None, 0))

    def emit(self):
        nc = self.nc
        self.finish()
        keys = set()
        for e in self.ENGS:
            for waits, fn, inckey, incv in self.ops[e]:
                for key, val in waits:
                    keys.add(key)
                if inckey is not None:
                    keys.add(inckey)
        for key in sorted(keys, key=str):
            self.semobj[key] = self.es.enter_context(nc.semaphore("s_" + "_".join(str(k) for k in key)))
        self.n_sems = len(keys)
        with nc.Block() as block:
            for e in self.ENGS:
                ops = self.ops[e]
                if not ops:
                    continue

                def body(engine, ops=ops):
                    for waits, fn, inckey, incv in ops:
                        for key, val in waits:
                            engine.wait_ge(self.semobj[key], val)
                        if fn is not None:
                            ins = fn(engine)
                            if inckey is not None:
                                ins.then_inc(self.semobj[inckey], incv)

                getattr(block, self.EMAP[e])(body)
        self.es.close()

    def mm(self, out, terms):
        n = len(terms)
        for i, (l, r) in enumerate(terms):
            self.op("pe", lambda e, l=l, r=r, i=i: e.matmul(out, lhsT=l, rhs=r, start=(i == 0), stop=(i == n - 1)),
                    reads=[l, r], writes=[out], inc=(i == n - 1))

    def tr(self, out, in_, ident):
        self.op("pe", lambda e: e.transpose(out=out, in_=in_, identity=ident), reads=[in_, ident], writes=[out])

    def act(self, out, in_, func, bias=None, scale=None, eng="act"):
        kw = {}
        rd = [in_]
        if bias is not None:
            kw["bias"] = bias
            rd.append(bias)
        if scale is not None:
            kw["scale"] = scale
            rd.append(scale)
        self.op(eng, lambda e: e.activation(out=out, in_=in_, func=func, **kw), reads=rd, writes=[out])

    def tt(self, out, in0, in1, op, eng="dve"):
        self.op(eng, lambda e: e.tensor_tensor(out=out, in0=in0, in1=in1, op=op), reads=[in0, in1], writes=[out])

    def ts(self, out, in0, s1, op0, s2=None, op1=None, eng="dve"):
        if op1 is None:
            self.op(eng, lambda e: e.tensor_scalar(out=out, in0=in0, scalar1=s1, scalar2=None, op0=op0), reads=[in0, s1], writes=[out])
        else:
            self.op(eng, lambda e: e.tensor_scalar(out=out, in0=in0, scalar1=s1, scalar2=s2, op0=op0, op1=op1), reads=[in0, s1, s2], writes=[out])

    def stt(self, out, in0, scalar, in1, op0, op1, eng="dve"):
        self.op(eng, lambda e: e.scalar_tensor_tensor(out=out, in0=in0, scalar=scalar, in1=in1, op0=op0, op1=op1),
                reads=[in0, scalar, in1], writes=[out])

    def copy(self, out, in_, eng="dve"):
        if eng == "act":
            self.op("act", lambda e: e.activation(out=out, in_=in_, func=AF.Copy), reads=[in_], writes=[out])
        else:
            self.op(eng, lambda e: e.tensor_copy(out=out, in_=in_), reads=[in_], writes=[out])

    def memset(self, out, val, eng="dve"):
        self.op(eng, lambda e: e.memset(out, val), writes=[out])

    def scan(self, out, data0, data1, op0, op1, initial=0.0):
        self.op("dve", lambda e: e.tensor_tensor_scan(out=out, data0=data0, data1=data1, initial=initial, op0=op0, op1=op1),
                reads=[data0, data1], writes=[out])

    def rsum(self, out, in_):
        self.op("dve", lambda e: e.reduce_sum(out=out, in_=in_, axis=AX.X), reads=[in_], writes=[out])

    def recip(self, out, in_):
        self.op("dve", lambda e: e.reciprocal(out=out, in_=in_), reads=[in_], writes=[out])

    def aselect(self, out, in_, pattern, cmp, fill, base, cm):
        self.op("pool", lambda e: e.affine_select(out=out, in_=in_, pattern=pattern, compare_op=cmp, fill=fill, base=base, channel_multiplier=cm),
                reads=[in_], writes=[out])


EPS = 1e-6
GN_EPS = 64e-5
DECAY_SCALE = 0.6065306597
TT = 256
C = 128
NCH = TT // C

RW_COLS = 1024
FM_GLA = 0
FM_ML = 272
FM_SSD = 528
NFM = 1040
TMA = 0
TMB = 512
TMC = 1024
NTM = 1288
P64_RW = 0
P64_GLA = 20
P64_ML = 22
NP64 = 42
P128_G = 0
P128_SSD = 8
NP128 = 28
BC_GNG = 0; BC_GNB = 256; BC_GLAG = 512; BC_MLG = 640; BC_MLB = 896; BC_DTB = 900; BC_ALOG = 904; BC_D = 908
NBC = 912


def build_M(T, mixers=("rw", "gla", "ml", "ssd"), debug=False):
    nc = bass.Bass("TRN2", target_bir_lowering=False)
    kb = KB(nc)
    kb.debug = debug
    ntile = T // TT
    hT = kb.dram("hT", [1024, T], F32, "ExternalInput")
    w_rw = kb.dram("w_rw", [1024, RW_COLS], F32, "ExternalInput")
    mu_rw = kb.dram("mu_rw", [1, RW_COLS], F32, "ExternalInput")
    w_fm = kb.dram("w_fm", [1024, NFM], F32, "ExternalInput")
    w_tm = kb.dram("w_tm", [1024, NTM], F32, "ExternalInput")
    pp64d = kb.dram("pp64", [64, NP64], F32, "ExternalInput")
    pp128d = kb.dram("pp128", [128, NP128], F32, "ExternalInput")
    bcd = kb.dram("bc", [1, NBC], F32, "ExternalInput")
    lora64 = kb.dram("lora64", [64, 512], F32, "ExternalInput")
    gupd = kb.dram("gup", [128, 256], F32, "ExternalInput")
    fupd = kb.dram("fup", [16, 128], F32, "ExternalInput")
    yd = kb.dram("y", [T, 1024], BF16, "ExternalOutput")

    ident_f = kb.sb([128, 128], F32, "ident_f")
    ident_b = kb.sb([128, 128], BF16, "ident_b")
    triI = kb.sb([128, 128], F32, "triI")
    triSu = kb.sb([128, 128], F32, "triSu")
    triLo = kb.sb([128, 128], F32, "triLo")
    ones_f = kb.sb([128, 128], F32, "ones_f")
    ones_b = kb.sb([128, 128], BF16, "ones_b")
    zeros_f = kb.sb([128, 128], F32, "zeros_f")
    kb.memset(ones_f[:], 1.0, "pool")
    kb.memset(zeros_f[:], 0.0, "pool")
    kb.copy(ones_b[:], ones_f[:])
    for t_, pat, cm, cmp in ((ident_f, [[-1, 128]], 1, ALU.is_equal), (triI, [[1, 128]], -1, ALU.is_ge),
                             (triSu, [[1, 128]], -1, ALU.is_gt), (triLo, [[-1, 128]], 1, ALU.is_gt)):
        kb.aselect(t_[:], ones_f[:], pat, cmp, 0.0, 0, cm)
    kb.copy(ident_b[:], ident_f[:])

    pp64 = kb.sb([64, NP64], F32, "pp64s")
    pp128 = kb.sb([128, NP128], F32, "pp128s")
    bc = kb.sb([128, NBC], F32, "bcs")
    kb.dma("sp", pp64[:], pp64d[:])
    kb.dma("sp", pp128[:], pp128d[:])
    kb.dma("sp", bc[:], bcd[:].partition_broadcast(128).rearrange("p o n -> p (o n)"))
    omka = kb.sb([64, 4], F32, "omka")
    for h in range(4):
        kb.ts(omka[:, h:h + 1], pp64[:, P64_RW + 5 * h + 3:P64_RW + 5 * h + 4], -1.0, ALU.mult, 1.0, ALU.add)
    nfb = kb.sb([64, 2], F32, "nfb")
    kb.ts(nfb[:], pp64[:, P64_GLA:P64_GLA + 2], -1.0, ALU.mult)
    aneg = kb.sb([128, 4], F32, "aneg")
    kb.act(aneg[:], bc[:, BC_ALOG:BC_ALOG + 4], AF.Exp)
    kb.ts(aneg[:], aneg[:], -1.0, ALU.mult)
    wup_b = kb.sb([128, 512], BF16, "wup_b")
    kb.memset(wup_b[:], 0.0)
    gup_b = kb.sb([128, 256], BF16, "gup_b")
    fup_b = kb.sb([128, 128], BF16, "fup_b")
    kb.memset(fup_b[:], 0.0)
    kb.dma("pool", wup_b[0:64, :], lora64[:])
    kb.dma("pool", gup_b[:], gupd[:])
    kb.dma("pool", fup_b[0:16, :], fupd[:])

    wfm = kb.sb([128, 8, NFM], BF16, "wfm")
    wtm = kb.sb([128, 8, NTM], BF16, "wtm")
    wr1 = kb.sb([128, 8, RW_COLS], BF16, "wr1")
    wr2 = kb.sb([128, 8, RW_COLS], BF16, "wr2")
    pf = kb.rot(6, [128, 512], F32, "pf", psum=True)
    pb = kb.rot(2, [128, 1024], BF16, "pb", psum=True)
    hs_r = kb.rot(1, [128, 8, TT], F32, "hs")
    uT_r = kb.rot(2, [128, 8, TT + 3], BF16, "uT")
    sq_r = kb.rot(2, [128, TT], BF16, "sqr")
    rstd_r = kb.rot(1, [128, TT], F32, "rstd")
    ystage_r = kb.rot(2, [128, 1024], F32, "ystage")
    yso_r = kb.rot(2, [128, 1024], BF16, "yso")
    hTv = hT[:].rearrange("(c p) t -> p c t", p=128)
    hs0 = hs_r.tiles[0]
    mub = hs0[:, 0:4, :].rearrange("p a b -> p (a b)")
    omub = hs0[:, 4:8, :].rearrange("p a b -> p (a b)")
    kb.dma("sp", mub, mu_rw[:].partition_broadcast(128).rearrange("p o n -> p (o n)"))
    kb.ts(omub, mub, -1.0, ALU.mult, 1.0, ALU.add)
    for c in range(8):
        kb.dma("pool", wfm[:, c, :], w_fm[c * 128:(c + 1) * 128, :])
        kb.dma("pool", wtm[:, c, :], w_tm[c * 128:(c + 1) * 128, :])
        st = ystage_r.tiles[c % 2]
        kb.dma("sp", st[:], w_rw[c * 128:(c + 1) * 128, :])
        kb.tt(wr1[:, c, :], st[:], omub, ALU.mult)
        kb.tt(wr2[:, c, :], st[:], mub, ALU.mult, eng="pool")

    def R(n, shape, dt, name):
        return kb.rot(n, shape, dt, name)

    f512 = {p: R(2, [p, TT], F32, f"f512_{p}") for p in (64, 128)}

    uprev = [None]

    rw_H = [R(2, [128, 64], BF16, f"rwH{h}") for h in range(4)]
    rw_Hcur = []
    for h in range(4):
        for t_ in rw_H[h].tiles:
            kb.memset(t_[:], 0.0)
        rw_Hcur.append(rw_H[h].next())
    gl_Hf = [kb.sb([64, 128], F32, f"glHf{h}") for h in range(2)]
    gl_Hb = [R(2, [128, 128], BF16, f"glHb{h}") for h in range(2)]
    gl_Hcur = []
    for h in range(2):
        kb.memset(gl_Hf[h][:], 0.0)
        for t_ in gl_Hb[h].tiles:
            kb.memset(t_[:], 0.0)
        gl_Hcur.append(gl_Hb[h].next())
    ml_Cf = [kb.sb([64, 130], F32, f"mlCf{h}") for h in range(2)]
    ml_Cb = [R(2, [128, 130], BF16, f"mlCb{h}") for h in range(2)]
    ml_Ccur = []
    for h in range(2):
        kb.memset(ml_Cf[h][:], 0.0)
        for t_ in ml_Cb[h].tiles:
            kb.memset(t_[:], 0.0)
        ml_Ccur.append(ml_Cb[h].next())
    ml_pre = [kb.sb([64, TT + 3], F32, f"mlpre{g}") for g in range(4)]
    for g in range(4):
        kb.memset(ml_pre[g][:, 0:3], 0.0)
    sd_Sf = kb.sb([128, 256], F32, "sdSf")
    sd_Sb = R(2, [128, 256], BF16, "sdSb")
    kb.memset(sd_Sf[:], 0.0)
    sd_Scur = [sd_Sb.next()]
    kb.memset(sd_Scur[0][:], 0.0)
    sd_pre = [kb.sb([128, TT + 3], F32, f"sdpre{g}") for g in range(4)]
    for g in range(4):
        kb.memset(sd_pre[g][:, 0:3], 0.0)

    sm = {}

    def tmp(p, n, dt=F32, nm="s", k=2):
        key = (p, n, dt, nm)
        if key not in sm:
            sm[key] = R(k, [p, n], dt, f"{nm}_{p}_{n}_{len(sm)}")
        return sm[key].next()

    def tmpz(n, dt, nm, k=1):
        key = ("z", n, dt, nm)
        if key not in sm:
            sm[key] = R(k, [128, n], dt, f"{nm}_z{len(sm)}")
            for t_ in sm[key].tiles:
                kb.memset(t_[:], 0.0)
        return sm[key].next()

    def rowsum(out, in_sb, n):
        j = tmp(128, n, F32, "rsj")
        kb.scan(j[:], ones_f[:, 0:n], in_sb, ALU.mult, ALU.add)
        kb.copy(out, j[:, n - 1:n])

    def proj_fm(w, col0, ncols, uT, shift_w=None):
        ps = pf.next()
        terms = [(w[:, c, col0:col0 + ncols], uT[:, c, 3:3 + TT]) for c in range(8)]
        if shift_w is not None:
            terms += [(shift_w[:, c, col0:col0 + ncols], uT[:, c, 2:2 + TT]) for c in range(8)]
        kb.mm(ps[0:ncols, 0:TT], terms)
        return ps

    def rstd_from(out, ps_ap, n, eps, p=128):
        t = tmp(p, out.shape[-1], F32, "rs")
        kb.ts(t[:], ps_ap, 1.0 / n, ALU.mult, eps, ALU.add)
        kb.act(t[:], t[:], AF.Sqrt)
        kb.recip(out, t[:])

    def conv_silu(pre, wcols, p, out_ap, scale=None):
        acc = f512[p].next()
        kb.ts(acc[:], pre[:, 0:TT], wcols[:, 0:1], ALU.mult)
        for j in (1, 2, 3):
            kb.stt(acc[:], pre[:, j:j + TT], wcols[:, j:j + 1], acc[:], ALU.mult, ALU.add)
        kb.act(out_ap, acc[:], AF.Silu, bias=wcols[:, 4:5])
        if scale is not None:
            kb.ts(out_ap, out_ap, scale, ALU.mult)

    for ti in range(ntile):
        t0 = ti * TT
        hs = hs_r.next()
        kb.dma("sp", hs[:], hTv[:, :, t0:t0 + TT])
        pss = pf.next()
        for c in range(8):
            sq = sq_r.next()
            kb.act(sq[:], hs[:, c, :], AF.Square)
            kb.op("pe", lambda e, sq=sq, c=c, pss=pss: e.matmul(pss[:, 0:TT], lhsT=ones_b[:], rhs=sq[:], start=(c == 0), stop=(c == 7)),
                  reads=[ones_b, sq], writes=[pss], inc=True)
        rstd = rstd_r.next()
        rstd_from(rstd[:], pss[:, 0:TT], 1024.0, EPS)
        uT = uT_r.next()
        if uprev[0] is None:
            kb.memset(uT[:, :, 0:3], 0.0)
        else:
            kb.copy(uT[:, :, 0:3], uprev[0][:, :, TT:TT + 3])
        for c in range(8):
            kb.stt(uT[:, c, 3:3 + TT], hs[:, c, :], pp128[:, P128_G + c:P128_G + c + 1], rstd[:], ALU.mult, ALU.mult)
        uprev[0] = uT
        ys = [ystage_r.tiles[c] for c in range(NCH)]

        tma_sb = tmp(128, NCH * 512, F32, "tma", 1)
        tmb_sb = tmp(128, NCH * 512, F32, "tmb", 1)
        tmc_sb = tmp(128, NCH * 264, F32, "tmc", 1)
        for b in range(NCH):
            for (col0, n, dst) in ((TMA, 512, tma_sb), (TMB, 512, tmb_sb), (TMC, 264, tmc_sb)):
                ps = pf.next()
                kb.mm(ps[:, 0:n], [(uT[:, c, 3 + b * C:3 + (b + 1) * C], wtm[:, c, col0:col0 + n]) for c in range(8)])
                kb.copy(dst[:, b * n:(b + 1) * n], ps[:, 0:n], eng="act")

        if "rw" in mixers:
            ps = proj_fm(wr1, 768, 64, uT, wr2)
            tw = tmpz(TT, BF16, "tw")
            kb.act(tw[0:64, :], ps[0:64, 0:TT], AF.Tanh)
            ps = proj_fm(wr1, 832, 64, uT, wr2)
            al = tmpz(TT, BF16, "al")
            kb.copy(al[0:64, :], ps[0:64, 0:TT], eng="act")
            ps = proj_fm(wr1, 896, 128, uT, wr2)
            sg = tmp(128, TT, BF16, "sg", 1)
            kb.act(sg[:], ps[:, 0:TT], AF.Sigmoid)
            for h in range(4):
                pc = P64_RW + 5 * h
                w0, a0, k_k, k_a, r_k = (pp64[:, pc + i:pc + i + 1] for i in range(5))
                ps = proj_fm(wr1, h * 192, 64, uT, wr2)
                r_f = f512[64].next(); kb.copy(r_f[:], ps[0:64, 0:TT], eng="act")
                ps = proj_fm(wr1, h * 192 + 64, 64, uT, wr2)
                k_f = f512[64].next(); kb.copy(k_f[:], ps[0:64, 0:TT], eng="act")
                ps = proj_fm(wr1, h * 192 + 128, 64, uT, wr2)
                v_b = tmpz(TT, BF16, "v_b", 2); kb.copy(v_b[0:64, :], ps[0:64, 0:TT], eng="act")
                ps = pf.next(); kb.mm(ps[0:64, 0:TT], [(wup_b[:, h * 64:(h + 1) * 64], tw[:])])
                lw = tmp(64, TT, F32, "lw", 1)
                kb.act(lw[:], ps[0:64, 0:TT], AF.Sigmoid, bias=w0)
                kb.ts(lw[:], lw[:], -DECAY_SCALE, ALU.mult)
                ps = pf.next(); kb.mm(ps[0:64, 0:TT], [(wup_b[:, 256 + h * 64:256 + (h + 1) * 64], al[:])])
                a_f = tmp(64, TT, F32, "a_f", 1)
                kb.act(a_f[:], ps[0:64, 0:TT], AF.Sigmoid, bias=a0)
                A = tmp(64, TT, F32, "rwA", 1)
                B = tmp(64, TT, F32, "rwB", 1)
                kb.ts(A[:], k_f[:], k_k, ALU.mult)
                sqz = tmpz(TT, F32, "sqz"); kb.tt(sqz[0:64, :], A[:], A[:], ALU.mult)
                ps = pf.next(); kb.mm(ps[0:64, 0:TT], [(ones_f[:, 0:64], sqz[:])])
                kb.act(B[:], ps[0:64, 0:TT], AF.Sqrt)
                kb.ts(B[:], B[:], 1e-12, ALU.max)
                kb.recip(B[:], B[:])
                kb.tt(A[:], A[:], B[:], ALU.mult)
                kb.ts(B[:], a_f[:], k_a, ALU.mult, omka[:, h:h + 1], ALU.add)
                kb.tt(B[:], k_f[:], B[:], ALU.mult)
                kb.tt(a_f[:], a_f[:], A[:], ALU.mult)
                bt, kk, k2 = a_f, A, B
                prod = tmpz(TT, F32, "prod")
                kb.stt(prod[0:64, :], r_f[:], r_k, k2[:], ALU.mult, ALU.mult)
                L = tmp(64, TT, F32, "L", 1)
                for c in range(NCH):
                    kb.scan(L[:, c * C:(c + 1) * C], ones_f[0:64, 0:C], lw[:, c * C:(c + 1) * C], ALU.mult, ALU.add)
                eL = tmp(64, TT, F32, "eL", 1); kb.act(eL[:], L[:], AF.Exp)
                emL = tmp(64, TT, F32, "emL", 1); kb.act(emL[:], L[:], AF.Exp, scale=-1.0)
                Lm = tmp(64, TT, F32, "Lm", 1); kb.tt(Lm[:], L[:], lw[:], ALU.subtract)
                kb.act(Lm[:], Lm[:], AF.Exp)
                eR = lw
                for c in range(NCH):
                    lc = tmp(64, 1, F32, "lcb", 4); kb.copy(lc[:], L[:, (c + 1) * C - 1:(c + 1) * C])
                    kb.act(eR[:, c * C:(c + 1) * C], L[:, c * C:(c + 1) * C], AF.Exp, bias=lc[:], scale=-1.0)
                rp = tmpz(TT, BF16, "rp"); kb.tt(rp[0:64, :], r_f[:], eL[:], ALU.mult)
                kkp = tmpz(TT, BF16, "kkp"); kb.tt(kkp[0:64, :], kk[:], Lm[:], ALU.mult)
                km = tmpz(TT, BF16, "km"); kb.tt(km[0:64, :], k2[:], emL[:], ALU.mult)
                bm = tmpz(TT, BF16, "bm"); kb.tt(bm[0:64, :], bt[:], emL[:], ALU.mult)
                kt = tmpz(TT, BF16, "kt"); kb.tt(kt[0:64, :], k2[:], eR[:], ALU.mult)
                btn = tmpz(TT, BF16, "btn"); kb.stt(btn[0:64, :], bt[:], -1.0, eR[:], ALU.mult, ALU.mult)
                for c in range(NCH):
                    cs = slice(c * C, (c + 1) * C)
                    pt = pb.next()
                    for i, src in enumerate((kkp, kt, btn, v_b)):
                        kb.tr(pt[:, i * 128:(i + 1) * 128], src[:, cs], ident_b[:])
                    TK = tmp(128, 192, BF16, "TK")
                    ZK = tmp(128, 128, BF16, "ZK")
                    kb.copy(ZK[:, 0:64], pt[:, 0:64], eng="act")
                    kb.copy(TK[:].rearrange("p (a b) -> p a b", b=64), pt[:, 128:512].rearrange("p (a b) -> p a b", b=128)[:, :, 0:64])
                    Kt_tok, nBt_tok, V_tok = TK[:, 0:64], TK[:, 64:128], TK[:, 128:192]
                    ps = pf.next(); kb.mm(ps[:, 0:C], [(bm[:, cs], kkp[:, cs])])
                    Y = tmp(128, C, BF16, "Y", 3); kb.tt(Y[:], ps[:, 0:C], triSu[:], ALU.mult)
                    ps = pf.next(); kb.mm(ps[:, 0:C], [(kkp[:, cs], bm[:, cs])])
                    X = tmp(128, C, BF16, "X", 3); kb.tt(X[:], ps[:, 0:C], triLo[:], ALU.mult)
                    ps = pf.next(); kb.mm(ps[:, 0:C], [(km[:, cs], kkp[:, cs])])
                    AkvT = tmp(128, C, BF16, "AkvT"); kb.tt(AkvT[:], ps[:, 0:C], triSu[:], ALU.mult)
                    ps = pf.next(); kb.mm(ps[:, 0:C], [(km[:, cs], rp[:, cs])])
                    ArkT = tmp(128, C, BF16, "ArkT"); kb.tt(ArkT[:], ps[:, 0:C], triI[:], ALU.mult)
                    ps = pf.next(); kb.mm(ps[:, 0:C], [(bm[:, cs], rp[:, cs])])
                    nArbT = tmp(128, C, BF16, "nArbT"); kb.stt(nArbT[:], ps[:, 0:C], -1.0, triI[:], ALU.mult, ALU.mult)
                    Q = tmp(128, C, BF16, "Q", 3); kb.tt(Q[:], ident_f[:], Y[:], ALU.subtract)
                    for lev in range(1, 7):
                        psx = pf.next(); kb.mm(psx[:, 0:C], [(Y[:], X[:])])
                        Xn = tmp(128, C, BF16, "X", 3); kb.copy(Xn[:], psx[:, 0:C], eng="act")
                        if lev < 6:
                            psy = pf.next(); kb.mm(psy[:, 0:C], [(X[:], Y[:])])
                            Yn = tmp(128, C, BF16, "Y", 3); kb.copy(Yn[:], psy[:, 0:C], eng="act")
                        psq = pf.next(); kb.mm(psq[:, 0:C], [(Xn[:], Q[:])])
                        Qn = tmp(128, C, BF16, "Q", 3); kb.tt(Qn[:], psq[:, 0:C], Q[:], ALU.add)
                        X, Q = Xn, Qn
                        if lev < 6:
                            Y = Yn
                    ps = pf.next(); kb.mm(ps[:, 0:64], [(AkvT[:], V_tok)])
                    kb.copy(ZK[:, 64:128], ps[:, 0:64], eng="act")
                    ps = pf.next(); kb.mm(ps[:, 0:128], [(Q[:], ZK[:])])
                    WU = tmp(128, 128, BF16, "WU"); kb.copy(WU[:], ps[:, 0:128])
                    W_, U0 = WU[:, 0:64], WU[:, 64:128]
                    ps = pf.next(); kb.mm(ps[0:64, 0:C], [(W_, nArbT[:])])
                    QhT = tmpz(C, BF16, "QhT", 2); kb.tt(QhT[0:64, :], ps[0:64, 0:C], rp[0:64, cs], ALU.add)
                    Hc = rw_Hcur[h]
                    pO = pf.next(); kb.mm(pO[:, 0:64], [(ArkT[:], V_tok), (nArbT[:], U0), (QhT[:], Hc[:])])
                    ps = pf.next(); kb.mm(ps[0:64, 0:64], [(Kt_tok, V_tok), (nBt_tok, U0)])
                    F_sb = tmp(64, 64, F32, "Fsb"); kb.copy(F_sb[:], ps[0:64, 0:64], eng="act")
                    ps = pf.next(); kb.mm(ps[0:64, 0:64], [(W_, nBt_tok)])
                    GT = tmpz(64, BF16, "GT", 2)
                    kb.stt(GT[0:64, :], ident_f[0:64, 0:64], eL[:, (c + 1) * C - 1:(c + 1) * C], ps[0:64, 0:64], ALU.mult, ALU.add)
                    ps = pf.next(); kb.mm(ps[0:64, 0:64], [(GT[:], Hc[:])])
                    Hn = rw_H[h].next(); kb.tt(Hn[0:64, :], ps[0:64, 0:64], F_sb[:], ALU.add)
                    rw_Hcur[h] = Hn
                    o_rw = tmp(128, 64, F32, "orw"); kb.copy(o_rw[:], pO[:, 0:64], eng="act")
                    s1 = tmp(128, 1, F32, "s1"); rowsum(s1[:], o_rw[:], 64)
                    kb.ts(s1[:], s1[:], -1.0 / 64, ALU.mult)
                    cen = tmp(128, 64, F32, "cen"); kb.ts(cen[:], o_rw[:], s1[:], ALU.add)
                    sq3 = tmp(128, 64, F32, "sq3"); kb.tt(sq3[:], cen[:], cen[:], ALU.mult)
                    s2 = tmp(128, 1, F32, "s2"); rowsum(s2[:], sq3[:], 64)
                    rs = tmp(128, 1, F32, "rsd"); rstd_from(rs[:], s2[:], 64.0, GN_EPS)
                    y1 = tmp(128, 64, F32, "y1")
                    kb.stt(y1[:], cen[:], rs[:], bc[:, BC_GNG + h * 64:BC_GNG + (h + 1) * 64], ALU.mult, ALU.mult)
                    kb.tt(y1[:], y1[:], bc[:, BC_GNB + h * 64:BC_GNB + (h + 1) * 64], ALU.add)
                    psb = pf.next(); kb.mm(psb[:, 0:1], [(prod[:, cs], ones_f[:, 0:1])])
                    bon = tmp(128, 1, F32, "bon"); kb.copy(bon[:], psb[:, 0:1], eng="act")
                    kb.stt(y1[:], V_tok, bon[:], y1[:], ALU.mult, ALU.add)
                    psg = pf.next(); kb.mm(psg[:, 0:64], [(sg[:, cs], gup_b[:, h * 64:(h + 1) * 64])])
                    kb.tt(ys[c][:, h * 64:(h + 1) * 64], y1[:], psg[:, 0:64], ALU.mult)

        if "gla" in mixers:
            ps = proj_fm(wfm, FM_GLA + 256, 64, uT)
            flo_b = tmpz(TT, BF16, "flo"); kb.copy(flo_b[0:64, :], ps[0:64, 0:TT], eng="act")
            for h in range(2):
                ps = proj_fm(wfm, FM_GLA + h * 128, 64, uT)
                q_f = f512[64].next(); kb.copy(q_f[:], ps[0:64, 0:TT], eng="act")
                ps = proj_fm(wfm, FM_GLA + h * 128 + 64, 64, uT)
                k_f = f512[64].next(); kb.copy(k_f[:], ps[0:64, 0:TT], eng="act")
                ps = pf.next(); kb.mm(ps[0:64, 0:TT], [(fup_b[:, h * 64:(h + 1) * 64], flo_b[:])])
                la = tmp(64, TT, F32, "lw", 1)
                kb.act(la[:], ps[0:64, 0:TT], AF.Exp, bias=nfb[:, h:h + 1], scale=-1.0)
                kb.act(la[:], la[:], AF.Ln, bias=1.0)
                kb.ts(la[:], la[:], -1.0 / 16.0, ALU.mult)
                L = tmp(64, TT, F32, "L", 1)
                for c in range(NCH):
                    kb.scan(L[:, c * C:(c + 1) * C], ones_f[0:64, 0:C], la[:, c * C:(c + 1) * C], ALU.mult, ALU.add)
                eL = tmp(64, TT, F32, "eL", 1); kb.act(eL[:], L[:], AF.Exp)
                emL = tmp(64, TT, F32, "emL", 1); kb.act(emL[:], L[:], AF.Exp, scale=-1.0)
                eR = la
                for c in range(NCH):
                    lc = tmp(64, 1, F32, "lcb", 4); kb.copy(lc[:], L[:, (c + 1) * C - 1:(c + 1) * C])
                    kb.act(eR[:, c * C:(c + 1) * C], L[:, c * C:(c + 1) * C], AF.Exp, bias=lc[:], scale=-1.0)
                qd = tmpz(TT, BF16, "rp"); kb.stt(qd[0:64, :], q_f[:], 0.125, eL[:], ALU.mult, ALU.mult)
                km = tmpz(TT, BF16, "km"); kb.tt(km[0:64, :], k_f[:], emL[:], ALU.mult)
                kt = tmpz(TT, BF16, "kt"); kb.tt(kt[0:64, :], k_f[:], eR[:], ALU.mult)
                for c in range(NCH):
                    cs = slice(c * C, (c + 1) * C)
                    pt = pb.next(); kb.tr(pt[:, 0:128], kt[:, cs], ident_b[:])
                    Kt_tok = tmp(128, 64, BF16, "gKt"); kb.copy(Kt_tok[:], pt[:, 0:64])
                    V_tok = tmp(128, 128, BF16, "gV"); kb.copy(V_tok[:], tma_sb[:, c * 512 + h * 128:c * 512 + (h + 1) * 128], eng="act")
                    ps = pf.next(); kb.mm(ps[:, 0:C], [(km[:, cs], qd[:, cs])])
                    AT = tmp(128, C, BF16, "gAT"); kb.tt(AT[:], ps[:, 0:C], triI[:], ALU.mult)
                    Hc = gl_Hcur[h]
                    pO = pf.next(); kb.mm(pO[:, 0:128], [(AT[:], V_tok[:]), (qd[:, cs], Hc[:])])
                    ps = pf.next(); kb.mm(ps[0:64, 0:128], [(Kt_tok[:], V_tok[:])])
                    kb.stt(gl_Hf[h][:], gl_Hf[h][:], eL[:, (c + 1) * C - 1:(c + 1) * C], ps[0:64, 0:128], ALU.mult, ALU.add)
                    Hn = gl_Hb[h].next(); kb.copy(Hn[0:64, :], gl_Hf[h][:], eng="act")
                    gl_Hcur[h] = Hn
                    o_sb = tmp(128, 128, F32, "go"); kb.copy(o_sb[:], pO[:, 0:128], eng="act")
                    sq3 = tmp(128, 128, F32, "gsq"); kb.tt(sq3[:], o_sb[:], o_sb[:], ALU.mult)
                    s2 = tmp(128, 1, F32, "s2"); rowsum(s2[:], sq3[:], 128)
                    rs = tmp(128, 1, F32, "rsd"); rstd_from(rs[:], s2[:], 128.0, EPS)
                    kb.stt(o_sb[:], o_sb[:], rs[:], bc[:, BC_GLAG:BC_GLAG + 128], ALU.mult, ALU.mult)
                    sog = tmp(128, 128, F32, "gsog")
                    kb.act(sog[:], tma_sb[:, c * 512 + 256 + h * 128:c * 512 + 256 + (h + 1) * 128], AF.Silu)
                    kb.tt(ys[c][:, 256 + h * 128:256 + (h + 1) * 128], o_sb[:], sog[:], ALU.mult)

        if "ml" in mixers:
            qk_c = []
            for g in range(4):
                ps = proj_fm(wfm, FM_ML + g * 64, 64, uT)
                if ti > 0:
                    kb.copy(ml_pre[g][:, 0:3], ml_pre[g][:, TT:TT + 3])
                kb.copy(ml_pre[g][:, 3:3 + TT], ps[0:64, 0:TT], eng="act")
                o = tmpz(TT, BF16, f"mlqk{g}")
                conv_silu(ml_pre[g], pp64[:, P64_ML + 5 * g:P64_ML + 5 * g + 5], 64, o[0:64, :], scale=(0.125 if g % 2 == 1 else None))
                qk_c.append(o)
            for c in range(NCH):
                cs = slice(c * C, (c + 1) * C)
                ifp = tmc_sb[:, c * 264 + 256:c * 264 + 260]
                gts = tmp(128, 4, F32, "mlg")
                kb.tt(gts[:], ifp, bc[:, BC_MLB:BC_MLB + 4], ALU.add)
                ei = tmp(128, 2, F32, "mlei"); kb.act(ei[:], gts[:, 0:2], AF.Exp)
                a_tok = tmp(128, 2, F32, "mla")
                kb.act(a_tok[:], gts[:, 2:4], AF.Exp, scale=-1.0)
                kb.act(a_tok[:], a_tok[:], AF.Ln, bias=1.0)
                kb.ts(a_tok[:], a_tok[:], -1.0, ALU.mult)
                pA = pf.next()
                kb.mm(pA[:, 0:2], [(triI[:], a_tok[:])])
                kb.mm(pA[:, 2:4], [(triLo[:], a_tok[:])])
                kb.mm(pA[0:64, 4:6], [(ones_f[:, 0:64], a_tok[:])])
                eA = tmp(128, 6, F32, "mleA")
                kb.act(eA[:, 0:4], pA[:, 0:4], AF.Exp)
                kb.act(eA[0:64, 4:6], pA[0:64, 4:6], AF.Exp)
                for h in range(2):
                    qc, kc = qk_c[2 * h], qk_c[2 * h + 1]
                    lseg = tmp(128, C, F32, "lseg"); kb.ts(lseg[:], triLo[:], a_tok[:, h:h + 1], ALU.mult)
                    pS = pf.next(); kb.mm(pS[:, 0:C], [(lseg[:], triI[:])])
                    seg = tmp(128, C, F32, "seg"); kb.act(seg[:], pS[:, 0:C], AF.Exp)
                    kb.tt(seg[:], seg[:], triI[:], ALU.mult)
                    pQK = pf.next(); kb.mm(pQK[:, 0:C], [(kc[:, cs], qc[:, cs])])
                    att = tmp(128, C, BF16, "att"); kb.tt(att[:], pQK[:, 0:C], seg[:], ALU.mult)
                    vext = tmp(128, 130, BF16, "vext")
                    kb.memset(vext[:, 128:130], 0.0)
                    kb.ts(vext[:, 0:128], tmb_sb[:, c * 512 + h * 128:c * 512 + (h + 1) * 128], ei[:, h:h + 1], ALU.mult)
                    kb.copy(vext[:, 128:129], ei[:, h:h + 1])
                    vextw = tmp(128, 130, BF16, "vextw"); kb.ts(vextw[:], vext[:], eA[:, 2 + h:3 + h], ALU.mult)
                    Cc = ml_Ccur[h]
                    pN = pf.next(); kb.mm(pN[:, 0:130], [(att[:], vext[:])])
                    pI = pf.next(); kb.mm(pI[:, 0:130], [(qc[:, cs], Cc[:])])
                    tI = tmp(128, 130, F32, "mltI"); kb.ts(tI[:], pI[:, 0:130], eA[:, h:h + 1], ALU.mult)
                    num = tmp(128, 130, F32, "mlnum"); kb.tt(num[:], pN[:, 0:130], tI[:], ALU.add)
                    dd = tmp(128, 1, F32, "mldd"); kb.ts(dd[:], num[:, 128:129], -1.0, ALU.mult)
                    kb.tt(dd[:], dd[:], num[:, 128:129], ALU.max)
                    kb.ts(dd[:], dd[:], 1.0, ALU.max)
                    kb.recip(dd[:], dd[:])
                    hn = tmp(128, 128, F32, "mlhn"); kb.ts(hn[:], num[:, 0:128], dd[:], ALU.mult)
                    sq3 = tmp(128, 128, F32, "gsq"); kb.tt(sq3[:], hn[:], hn[:], ALU.mult)
                    s2 = tmp(128, 1, F32, "s2"); rowsum(s2[:], sq3[:], 128)
                    rs = tmp(128, 1, F32, "rsd"); rstd_from(rs[:], s2[:], 128.0, EPS)
                    kb.stt(hn[:], hn[:], rs[:], bc[:, BC_MLG + h * 128:BC_MLG + (h + 1) * 128], ALU.mult, ALU.mult)
                    sog = tmp(128, 128, F32, "gsog")
                    kb.act(sog[:], tmb_sb[:, c * 512 + 256 + h * 128:c * 512 + 256 + (h + 1) * 128], AF.Sigmoid)
                    kb.tt(ys[c][:, 512 + h * 128:512 + (h + 1) * 128], hn[:], sog[:], ALU.mult)
                    pt = pb.next(); kb.tr(pt[:, 0:128], kc[:, cs], ident_b[:])
                    k_tok = tmp(128, 64, BF16, "gKt"); kb.copy(k_tok[:], pt[:, 0:64])
                    pC = pf.next(); kb.mm(pC[0:64, 0:130], [(k_tok[:], vextw[:])])
                    kb.stt(ml_Cf[h][:], ml_Cf[h][:], eA[0:64, 4 + h:5 + h], pC[0:64, 0:130], ALU.mult, ALU.add)
                    Cn = ml_Cb[h].next(); kb.copy(Cn[0:64, :], ml_Cf[h][:], eng="act")
                    ml_Ccur[h] = Cn

        if "ssd" in mixers:
            sd_c = []
            for g in range(4):
                ps = proj_fm(wfm, FM_SSD + g * 128, 128, uT)
                if ti > 0:
                    kb.copy(sd_pre[g][:, 0:3], sd_pre[g][:, TT:TT + 3])
                kb.copy(sd_pre[g][:, 3:3 + TT], ps[:, 0:TT], eng="act")
                o = tmp(128, TT, F32 if g < 2 else BF16, f"sdc{g}", 1)
                conv_silu(sd_pre[g], pp128[:, P128_SSD + 5 * g:P128_SSD + 5 * g + 5], 128, o[:])
                sd_c.append(o)
            bcast = lambda ap: ap.unsqueeze(2).to_broadcast([128, 4, 64])
            v3 = lambda ap: ap.rearrange("p (h d) -> p h d", h=4)
            for c in range(NCH):
                cs = slice(c * C, (c + 1) * C)
                pX = pf.next()
                kb.tr(pX[:, 0:128], sd_c[0][:, cs], ident_f[:])
                kb.tr(pX[:, 128:256], sd_c[1][:, cs], ident_f[:])
                x_tok = tmp(128, 256, F32, "sdx"); kb.copy(x_tok[:], pX[:, 0:256], eng="act")
                pt = pb.next(); kb.tr(pt[:, 0:128], sd_c[2][:, cs], ident_b[:])
                B_tok = tmp(128, 128, BF16, "sdB"); kb.copy(B_tok[:], pt[:, 0:128])
                dt = tmp(128, 4, F32, "sddt")
                kb.tt(dt[:], tmc_sb[:, c * 264 + 260:c * 264 + 264], bc[:, BC_DTB:BC_DTB + 4], ALU.add)
                kb.act(dt[:], dt[:], AF.Exp)
                kb.act(dt[:], dt[:], AF.Ln, bias=1.0)
                a_tok = tmp(128, 4, F32, "sda"); kb.tt(a_tok[:], dt[:], aneg[:], ALU.mult)
                pA = pf.next()
                kb.mm(pA[:, 0:4], [(triI[:], a_tok[:])])
                kb.mm(pA[:, 4:8], [(triLo[:], a_tok[:])])
                kb.mm(pA[:, 8:12], [(ones_f[:], a_tok[:])])
                eA = tmp(128, 12, F32, "sdeA"); kb.act(eA[:], pA[:, 0:12], AF.Exp)
                kb.dump("d_bc", bc[:]); kb.dump("d_dt", dt[:]); kb.dump("d_xtok", x_tok[:]); kb.dump("d_eA", eA[:]); kb.dump("d_atok", a_tok[:]); kb.dump("d_aneg", aneg[:])
                kb.dump("d_sdc0", sd_c[0][:]); kb.dump("d_tmc", tmc_sb[:])
                xdt = tmp(128, 256, BF16, "sdxdt"); kb.tt(v3(xdt[:]), v3(x_tok[:]), bcast(dt[:]), ALU.mult)
                xdtw = tmp(128, 256, BF16, "sdxdtw"); kb.tt(v3(xdtw[:]), v3(xdt[:]), bcast(eA[:, 4:8]), ALU.mult)
                pCB = pf.next(); kb.mm(pCB[:, 0:C], [(sd_c[2][:, cs], sd_c[3][:, cs])])
                CBm = tmp(128, C, F32, "sdCB"); kb.tt(CBm[:], pCB[:, 0:C], triI[:], ALU.mult)
                pY1 = pf.next()
                for h in range(4):
                    lseg = tmp(128, C, F32, "lseg"); kb.ts(lseg[:], triLo[:], a_tok[:, h:h + 1], ALU.mult)
                    pS = pf.next(); kb.mm(pS[:, 0:C], [(lseg[:], triI[:])])
                    seg = tmp(128, C, F32, "seg"); kb.act(seg[:], pS[:, 0:C], AF.Exp)
                    att = tmp(128, C, BF16, "att"); kb.tt(att[:], CBm[:], seg[:], ALU.mult)
                    kb.mm(pY1[:, h * 64:(h + 1) * 64], [(att[:], xdt[:, h * 64:(h + 1) * 64])])
                Sc = sd_Scur[0]
                pY2 = pf.next(); kb.mm(pY2[:, 0:256], [(sd_c[3][:, cs], Sc[:])])
                y = tmp(128, 256, F32, "sdy")
                kb.tt(v3(y[:]), v3(pY2[:, 0:256]), bcast(eA[:, 0:4]), ALU.mult)
                kb.tt(y[:], y[:], pY1[:, 0:256], ALU.add)
                xd = tmp(128, 256, F32, "sdxd"); kb.tt(v3(xd[:]), v3(x_tok[:]), bcast(bc[:, BC_D:BC_D + 4]), ALU.mult)
                kb.tt(y[:], y[:], xd[:], ALU.add)
                sz = tmp(128, 256, F32, "sdsz"); kb.act(sz[:], tmc_sb[:, c * 264:c * 264 + 256], AF.Silu)
                kb.tt(ys[c][:, 768:1024], y[:], sz[:], ALU.mult)
                pS2 = pf.next(); kb.mm(pS2[:, 0:256], [(B_tok[:], xdtw[:])])
                kb.tt(v3(sd_Sf[:]), v3(sd_Sf[:]), bcast(eA[:, 8:12]), ALU.mult)
                kb.tt(sd_Sf[:], sd_Sf[:], pS2[:, 0:256], ALU.add)
                Sn = sd_Sb.next(); kb.copy(Sn[:], sd_Sf[:], eng="act")
                sd_Scur[0] = Sn

        for c in range(NCH):
            yo = yso_r.next()
            kb.copy(yo[:], ys[c][:], eng="act")
            kb.dma("sp", yd[t0 + c * C:t0 + (c + 1) * C, :], yo[:])

    kb.emit()
    return nc


EPS = 1e-6
DW = 256
NFF = 22
PD_GMIX = 0; PD_GFFN = 8; PD_GFIN = 16; PD_SNG = 24; PD_CONV = 28
NPD = 28 + 44 * 4


def build_P(shapes):
    nc = bass.Bass("TRN2", target_bir_lowering=False)
    kb = KB(nc)
    st = kb.rot(4, [128, 2048], BF16, "pst")
    for name, (rows, cols) in shapes.items():
        src = kb.dram(name, [rows, cols], F32, "ExternalInput")
        dst = kb.dram(name + "_b", [rows, cols], BF16, "ExternalOutput")
        for r0 in range(0, rows, 128):
            nr = min(128, rows - r0)
            for c0 in range(0, cols, 2048):
                ncol = min(2048, cols - c0)
                t = st.next()
                kb.dma("pool", t[0:nr, 0:ncol], src[r0:r0 + nr, c0:c0 + ncol])
                kb.dma("sp", dst[r0:r0 + nr, c0:c0 + ncol], t[0:nr, 0:ncol])
    kb.emit()
    return nc


def build_D(NTOK):
    nc = bass.Bass("TRN2", target_bir_lowering=False)
    kb = KB(nc)
    NC = NTOK + 2
    hT = kb.dram("hT", [1024, NC], F32, "ExternalInput")
    yT = kb.dram("yT", [2048, NC], BF16, "ExternalInput")
    wg = kb.dram("w_gate", [1024, 4096], BF16, "ExternalInput")
    bp = kb.dram("bp", [2048, 1024], BF16, "ExternalInput")
    wo = kb.dram("w_out", [1024, 1024], BF16, "ExternalInput")
    fu = kb.dram("ffn_up", [1024, 5632], BF16, "ExternalInput")
    fd = kb.dram("ffn_down", [2816, 1024], BF16, "ExternalInput")
    pDd = kb.dram("pD", [128, NPD], F32, "ExternalInput")
    hout = kb.dram("hout", [1024, NTOK], F32, "ExternalOutput")
    hfin = kb.dram("hfin", [1024, NTOK], F32, "ExternalOutput")
    hTv = hT[:].rearrange("(c p) t -> p c t", p=128)
    yTv = yT[:].rearrange("(c p) t -> p c t", p=128)
    wgv = wg[:].rearrange("(c p) n -> p c n", p=128)
    bpv = bp[:].rearrange("(c p) n -> p c n", p=128)
    wov = wo[:].rearrange("(c p) n -> p c n", p=128)
    fuv = fu[:].rearrange("(c p) n -> p c n", p=128)
    fdv = fd[:].rearrange("(c p) n -> p c n", p=128)
    houtv = hout[:].rearrange("(c p) t -> p c t", p=128)
    hfinv = hfin[:].rearrange("(c p) t -> p c t", p=128)

    ones_b = kb.sb([128, 128], BF16, "ones_b")
    kb.memset(ones_b[:], 1.0)
    pD = kb.sb([128, NPD], F32, "pDs")
    kb.dma("sp", pD[:], pDd[:])
    carry = kb.sb([128, 44, 2], F32, "carry")
    kb.memset(carry[:], 0.0)

    pf = kb.rot(8, [128, 512], F32, "pf", psum=True)
    hs_r = kb.rot(1, [128, 8, DW], F32, "hs")
    ystg_r = kb.rot(2, [128, 4, DW], BF16, "ystg")
    yb_r = kb.rot(1, [128, 16, DW], BF16, "yb")
    uT_r = kb.rot(1, [128, 8, DW], BF16, "uT")
    mf_r = kb.rot(1, [128, 8, DW], F32, "mf")
    mb_r = kb.rot(1, [128, 8, DW], BF16, "mb")
    u2_r = kb.rot(1, [128, 8, DW], BF16, "u2")
    a_r = kb.rot(1, [128, NFF, DW], BF16, "a")
    hf_r = kb.rot(1, [128, 8, DW], F32, "hf")
    zb_r = kb.rot(3, [128, DW + 2], F32, "zb")
    t_r = kb.rot(6, [128, DW], F32, "tt")
    sq_r = kb.rot(2, [128, DW], BF16, "sq")
    rs_r = kb.rot(2, [128, DW], F32, "rs")
    wgi_r = kb.rot(2, [128, 8, 1024], BF16, "wgi")
    bpi_r = kb.rot(2, [128, 4, 1024], BF16, "bpi")
    wo_r = kb.rot(1, [128, 8, 1024], BF16, "wo")
    wu_r = kb.rot(4, [128, 8, 256], BF16, "wu")
    wd_r = kb.rot(1, [128, NFF, 1024], BF16, "wd")

    def rmsnorm(src, gcol, nch, dst, W, n):
        ps = pf.next()
        for c in range(nch):
            sq = sq_r.next()
            kb.act(sq[:, 0:W], src(c), AF.Square)
            kb.op("pe", lambda e, sq=sq, c=c, ps=ps: e.matmul(ps[:, 0:W], lhsT=ones_b[:], rhs=sq[:, 0:W], start=(c == 0), stop=(c == nch - 1)),
                  reads=[ones_b, sq], writes=[ps])
        rs = rs_r.next()
        kb.ts(rs[:, 0:W], ps[:, 0:W], 1.0 / n, ALU.mult, EPS, ALU.add)
        kb.act(rs[:, 0:W], rs[:, 0:W], AF.Sqrt)
        kb.recip(rs[:, 0:W], rs[:, 0:W])
        for c in range(nch):
            kb.stt(dst(c), src(c), pD[:, gcol + c:gcol + c + 1], rs[:, 0:W], ALU.mult, ALU.mult)

    def d_tile(col0, W, halo):
        hs = hs_r.next()
        kb.dma("sp", hs[:, :, 0:W], hTv[:, :, col0:col0 + W])
        yb = yb_r.next()
        kb.dma("sp", yb[:, 0:12, 0:W], yTv[:, 0:12, col0:col0 + W])
        ystg = ystg_r.next()
        kb.dma("sp", ystg[:, :, 0:W], yTv[:, 12:16, col0:col0 + W])
        rmsnorm(lambda c: ystg[:, c, 0:W], PD_SNG, 4, lambda c: yb[:, 12 + c, 0:W], W, 512.0)
        uT = uT_r.next()
        rmsnorm(lambda c: hs[:, c, 0:W], PD_GMIX, 8, lambda c: uT[:, c, 0:W], W, 1024.0)
        mf = mf_r.next()
        mb = mb_r.next()
        for i in range(4):
            wgi = wgi_r.next()
            kb.dma("sp", wgi[:], wgv[:, :, i * 1024:(i + 1) * 1024])
            bpi = bpi_r.next()
            kb.dma("sp", bpi[:], bpv[:, i * 4:(i + 1) * 4, :])
            for m in range(8):
                ms = slice(m * 128, (m + 1) * 128)
                pg = pf.next()
                kb.mm(pg[:, 0:W], [(wgi[:, kc, ms], uT[:, kc, 0:W]) for kc in range(8)])
                gsb = t_r.next()
                kb.act(gsb[:, 0:W], pg[:, 0:W], AF.Sigmoid)
                pp = pf.next()
                kb.mm(pp[:, 0:W], [(bpi[:, c4, ms], yb[:, i * 4 + c4, 0:W]) for c4 in range(4)])
                if i == 0:
                    kb.tt(mf[:, m, 0:W], gsb[:, 0:W], pp[:, 0:W], ALU.mult)
                else:
                    kb.tt(gsb[:, 0:W], gsb[:, 0:W], pp[:, 0:W], ALU.mult)
                    if i < 3:
                        kb.tt(mf[:, m, 0:W], mf[:, m, 0:W], gsb[:, 0:W], ALU.add)
                    else:
                        kb.tt(mb[:, m, 0:W], mf[:, m, 0:W], gsb[:, 0:W], ALU.add)
        wot = wo_r.next()
        kb.dma("sp", wot[:], wov)
        for m in range(8):
            ms = slice(m * 128, (m + 1) * 128)
            ps = pf.next()
            kb.mm(ps[:, 0:W], [(wot[:, kc, ms], mb[:, kc, 0:W]) for kc in range(8)])
            kb.tt(hs[:, m, 0:W], hs[:, m, 0:W], ps[:, 0:W], ALU.add)
        u2 = u2_r.next()
        rmsnorm(lambda c: hs[:, c, 0:W], PD_GFFN, 8, lambda c: u2[:, c, 0:W], W, 1024.0)
        a = a_r.next()
        for blk in range(NFF // 2):
            wug = wu_r.next()
            kb.dma("sp", wug[:], fuv[:, :, blk * 256:(blk + 1) * 256])
            wuv = wu_r.next()
            kb.dma("sp", wuv[:], fuv[:, :, 2816 + blk * 256:2816 + (blk + 1) * 256])
            for j in range(2):
                f = blk * 2 + j
                res = []
                for (wsrc, fc) in ((wug, f), (wuv, NFF + f)):
                    ps = pf.next()
                    kb.mm(ps[:, 0:W], [(wsrc[:, kc, j * 128:(j + 1) * 128], u2[:, kc, 0:W]) for kc in range(8)])
                    zb = zb_r.next()
                    kb.copy(zb[:, 0:2], carry[:, fc, :])
                    kb.copy(zb[:, 2:2 + W], ps[:, 0:W], eng="act")
                    kb.copy(carry[:, fc, :], zb[:, W:W + 2])
                    if halo:
                        continue
                    pc = PD_CONV + 4 * fc
                    cv = t_r.next()
                    kb.ts(cv[:, 0:W], zb[:, 0:W], pD[:, pc:pc + 1], ALU.mult, pD[:, pc + 3:pc + 4], ALU.add)
                    kb.stt(cv[:, 0:W], zb[:, 1:1 + W], pD[:, pc + 1:pc + 2], cv[:, 0:W], ALU.mult, ALU.add)
                    kb.stt(cv[:, 0:W], zb[:, 2:2 + W], pD[:, pc + 2:pc + 3], cv[:, 0:W], ALU.mult, ALU.add)
                    res.append(cv)
                if halo:
                    continue
                sg = t_r.next()
                kb.act(sg[:, 0:W], res[0][:, 0:W], AF.Silu)
                kb.tt(a[:, f, 0:W], sg[:, 0:W], res[1][:, 0:W], ALU.mult)
        if halo:
            return
        wd = wd_r.next()
        kb.dma("sp", wd[:], fdv)
        for m in range(8):
            ms = slice(m * 128, (m + 1) * 128)
            ps = pf.next()
            kb.mm(ps[:, 0:W], [(wd[:, f, ms], a[:, f, 0:W]) for f in range(NFF)])
            kb.tt(hs[:, m, 0:W], hs[:, m, 0:W], ps[:, 0:W], ALU.add)
        kb.dma("pool", houtv[:, :, col0 - 2:col0 - 2 + W], hs[:, :, 0:W])
        hf = hf_r.next()
        rmsnorm(lambda c: hs[:, c, 0:W], PD_GFIN, 8, lambda c: hf[:, c, 0:W], W, 1024.0)
        kb.dma("pool", hfinv[:, :, col0 - 2:col0 - 2 + W], hf[:, :, 0:W])

    d_tile(0, 2, True)
    for ti in range(NTOK // DW):
        d_tile(2 + ti * DW, DW, False)
    kb.emit()
    return nc


import numpy as np

OFF_RW, OFF_GLA, OFF_ML, OFF_SSD, OFF_GATE = 0, 1792, 3344, 4888, 6432


def m_inputs(P, l, j):
    w_in = P['w_in'][l]
    f32 = np.float32
    rw_idx = []
    for hh in range(4):
        H = 4 * j + hh
        for blk in range(3):
            rw_idx += list(range(blk * 512 + H * 64, blk * 512 + (H + 1) * 64))
    rw_idx += list(range(1536, 1792))
    rw_idx = np.array(rw_idx)
    w_rw = np.ascontiguousarray(w_in[:, OFF_RW + rw_idx])
    mu_rw = np.ascontiguousarray(P['rwkv_mu'][l][rw_idx][None, :])
    fm = []
    for hh in range(2):
        Hg = 2 * j + hh
        fm += list(range(OFF_GLA + Hg * 64, OFF_GLA + (Hg + 1) * 64))
        fm += list(range(OFF_GLA + 256 + Hg * 64, OFF_GLA + 256 + (Hg + 1) * 64))
    fm += list(range(OFF_GLA + 1024, OFF_GLA + 1040))
    ml_ch = []
    for hh in range(2):
        Hm = 2 * j + hh
        ml_ch.append(np.arange(Hm * 64, (Hm + 1) * 64))
        ml_ch.append(np.arange(256 + Hm * 64, 256 + (Hm + 1) * 64))
    for ch in ml_ch:
        fm += list(OFF_ML + ch)
    sd_ch = [np.arange(j * 256, j * 256 + 128), np.arange(j * 256 + 128, j * 256 + 256),
             np.arange(512 + j * 128, 512 + (j + 1) * 128), np.arange(768 + j * 128, 768 + (j + 1) * 128)]
    for ch in sd_ch:
        fm += list(OFF_SSD + 512 + ch)
    w_fm = np.ascontiguousarray(w_in[:, np.array(fm)])
    assert w_fm.shape[1] == NFM
    tm = []
    for hh in range(2):
        Hg = 2 * j + hh
        tm += list(range(OFF_GLA + 512 + Hg * 128, OFF_GLA + 512 + (Hg + 1) * 128))
    for hh in range(2):
        Hg = 2 * j + hh
        tm += list(range(OFF_GLA + 1040 + Hg * 128, OFF_GLA + 1040 + (Hg + 1) * 128))
    tm += list(range(OFF_ML + 512 + j * 256, OFF_ML + 512 + (j + 1) * 256))
    tm += list(range(OFF_ML + 1032 + j * 256, OFF_ML + 1032 + (j + 1) * 256))
    tm += list(range(OFF_SSD + j * 256, OFF_SSD + (j + 1) * 256))
    tm += [OFF_ML + 1024 + 2 * j, OFF_ML + 1024 + 2 * j + 1, OFF_ML + 1028 + 2 * j, OFF_ML + 1028 + 2 * j + 1]
    tm += list(range(OFF_SSD + 1536 + 4 * j, OFF_SSD + 1536 + 4 * j + 4))
    w_tm = np.ascontiguousarray(w_in[:, np.array(tm)])
    assert w_tm.shape[1] == NTM
    pp64 = np.zeros((64, NP64), f32)
    for hh in range(4):
        H = 4 * j + hh
        sl = slice(H * 64, (H + 1) * 64)
        pp64[:, 5 * hh + 0] = P['rwkv_w0'][l][sl]
        pp64[:, 5 * hh + 1] = P['rwkv_a0'][l][sl]
        pp64[:, 5 * hh + 2] = P['rwkv_k_k'][l][sl]
        pp64[:, 5 * hh + 3] = P['rwkv_k_a'][l][sl]
        pp64[:, 5 * hh + 4] = P['rwkv_r_k'][l][H]
    for hh in range(2):
        Hg = 2 * j + hh
        pp64[:, P64_GLA + hh] = P['gla_f_bias'][l][Hg * 64:(Hg + 1) * 64]
    for g, ch in enumerate(ml_ch):
        pp64[:, P64_ML + 5 * g:P64_ML + 5 * g + 4] = P['mlstm_conv_w'][l][:, ch].T
        pp64[:, P64_ML + 5 * g + 4] = P['mlstm_conv_b'][l][ch]
    pp128 = np.zeros((128, NP128), f32)
    pp128[:, 0:8] = P['mix_norm_g'][l].reshape(8, 128).T
    for g, ch in enumerate(sd_ch):
        pp128[:, P128_SSD + 5 * g:P128_SSD + 5 * g + 4] = P['ssd_conv_w'][l][:, ch].T
        pp128[:, P128_SSD + 5 * g + 4] = P['ssd_conv_b'][l][ch]
    bc = np.zeros((1, NBC), f32)
    bc[0, BC_GNG:BC_GNG + 256] = P['rwkv_gn_g'][l][j * 256:(j + 1) * 256]
    bc[0, BC_GNB:BC_GNB + 256] = P['rwkv_gn_b'][l][j * 256:(j + 1) * 256]
    bc[0, BC_GLAG:BC_GLAG + 128] = P['gla_norm_g'][l]
    bc[0, BC_MLG:BC_MLG + 256] = P['mlstm_norm_g'][l][j * 256:(j + 1) * 256]
    bc[0, BC_MLB:BC_MLB + 2] = P['mlstm_i_bias'][l][2 * j:2 * j + 2]
    bc[0, BC_MLB + 2:BC_MLB + 4] = P['mlstm_f_bias'][l][2 * j:2 * j + 2]
    bc[0, BC_DTB:BC_DTB + 4] = P['ssd_dt_bias'][l][4 * j:4 * j + 4]
    bc[0, BC_ALOG:BC_ALOG + 4] = P['ssd_a_log'][l][4 * j:4 * j + 4]
    bc[0, BC_D:BC_D + 4] = P['ssd_d'][l][4 * j:4 * j + 4]
    lora64 = np.ascontiguousarray(np.concatenate([P['rwkv_w_up'][l][:, j * 256:(j + 1) * 256],
                                                  P['rwkv_a_up'][l][:, j * 256:(j + 1) * 256]], axis=1))
    gup = np.ascontiguousarray(P['rwkv_g_up'][l][:, j * 256:(j + 1) * 256])
    fup = np.ascontiguousarray(P['gla_f_up'][l][:, j * 128:(j + 1) * 128])
    return dict(w_rw=w_rw, mu_rw=mu_rw, w_fm=w_fm, w_tm=w_tm, pp64=pp64, pp128=pp128, bc=bc,
                lora64=lora64, gup=gup, fup=fup)


import ml_dtypes
_CACHE = {}


def d_params(P, l):
    pD = np.zeros((128, NPD), np.float32)
    pD[:, PD_GMIX:PD_GMIX + 8] = P['mix_norm_g'][l].reshape(8, 128).T
    pD[:, PD_GFFN:PD_GFFN + 8] = P['ffn_norm_g'][l].reshape(8, 128).T
    pD[:, PD_GFIN:PD_GFIN + 8] = P['final_norm_g'].reshape(8, 128).T
    pD[:, PD_SNG:PD_SNG + 4] = P['ssd_norm_g'][l].reshape(4, 128).T
    cw = P['ffn_conv_w'][l].reshape(3, 44, 128)
    cb = P['ffn_conv_b'][l].reshape(44, 128)
    for fc in range(44):
        pD[:, PD_CONV + 4 * fc:PD_CONV + 4 * fc + 3] = cw[:, fc, :].T
        pD[:, PD_CONV + 4 * fc + 3] = cb[fc]
    return pD


def kernel(**inputs):
    P = {k: np.asarray(v) for k, v in inputs.items()}
    x = P['x']
    B, T, D = x.shape
    L = P['w_in'].shape[0]
    NCORE = 8
    cores = list(range(NCORE))
    NTOK = (B * T) // NCORE
    per_b = T // NTOK
    wsrc = {
        'w_gate': [P['w_in'][l][:, OFF_GATE:] for l in range(L)],
        'bp': [P['branch_proj'][l].reshape(2048, 1024) for l in range(L)],
        'w_out': [P['w_out'][l] for l in range(L)],
        'ffn_up': [P['ffn_up'][l] for l in range(L)],
        'ffn_down': [P['ffn_down'][l] for l in range(L)],
    }
    shapes = {}
    in_maps = [dict() for _ in cores]
    for name, mats in wsrc.items():
        rows = mats[0].shape[0] // NCORE
        shapes[name] = (L * rows, mats[0].shape[1])
        for k in cores:
            in_maps[k][name] = np.ascontiguousarray(np.concatenate([m[k * rows:(k + 1) * rows] for m in mats], axis=0))
    if 'P' not in _CACHE:
        _CACHE['P'] = build_P(shapes)
    res = run_bass_kernel_spmd(_CACHE['P'], in_maps, core_ids=cores)
    wb = {}
    for name, mats in wsrc.items():
        rows = mats[0].shape[0] // NCORE
        wb[name] = [np.ascontiguousarray(np.concatenate([np.asarray(res.results[k][name + '_b'])[l * rows:(l + 1) * rows] for k in cores], axis=0))
                    for l in range(L)]
    del res, in_maps
    if 'M' not in _CACHE:
        _CACHE['M'] = build_M(T)
    if 'D' not in _CACHE:
        _CACHE['D'] = build_D(NTOK)
    h = x.astype(np.float32, copy=True)
    out = None
    for l in range(L):
        in_maps = []
        mi = [m_inputs(P, l, j) for j in range(2)]
        for k in cores:
            b, j = k // 2, k % 2
            d = dict(mi[j])
            d['hT'] = np.ascontiguousarray(h[b].T)
            in_maps.append(d)
        res = run_bass_kernel_spmd(_CACHE['M'], in_maps, core_ids=cores)
        ybr = np.empty((B, T, 2048), ml_dtypes.bfloat16)
        for k in cores:
            b, j = k // 2, k % 2
            y = np.asarray(res.results[k]['y'])
            for i in range(4):
                ybr[b, :, i * 512 + j * 256:i * 512 + (j + 1) * 256] = y[:, i * 256:(i + 1) * 256]
        del res
        in_maps = []
        pD = d_params(P, l)
        for k in cores:
            b, part = k // per_b, k % per_b
            s = part * NTOK
            hT = np.zeros((1024, NTOK + 2), np.float32)
            yT = np.zeros((2048, NTOK + 2), ml_dtypes.bfloat16)
            lo = max(s - 2, 0)
            hT[:, 2 - (s - lo):] = h[b, lo:s + NTOK].T
            yT[:, 2 - (s - lo):] = ybr[b, lo:s + NTOK].T
            in_maps.append(dict(hT=hT, yT=yT, w_gate=wb['w_gate'][l], bp=wb['bp'][l], w_out=wb['w_out'][l],
                                ffn_up=wb['ffn_up'][l], ffn_down=wb['ffn_down'][l], pD=pD))
        res = run_bass_kernel_spmd(_CACHE['D'], in_maps, core_ids=cores)
        key = 'hfin' if l == L - 1 else 'hout'
        hn = np.empty_like(h)
        for k in cores:
            b, part = k // per_b, k % per_b
            s = part * NTOK
            hn[b, s:s + NTOK] = np.asarray(res.results[k][key]).T
        h = hn
        del res, in_maps
    return h.astype(x.dtype)
```
